# Optimizing a Trainium2 kernel written in Bass

```python
import jax
import jax.numpy as jnp
from jax import lax
import numpy as np

D_MODEL = 2048
BATCH = 2
SEQ = 8192
DEPTH = 1

F32 = jnp.float32
ATTN_HEADS = 16
ATTN_HEAD_DIM = 64
ATTN_WIDTH = ATTN_HEADS * ATTN_HEAD_DIM
HGRN_HEADS = 8
HGRN_HEAD_DIM = 128
HGRN_WIDTH = HGRN_HEADS * HGRN_HEAD_DIM
MIX_WIDTH = ATTN_WIDTH + HGRN_WIDTH
IN_PROJ_WIDTH = 3 * ATTN_WIDTH + 5 * HGRN_WIDTH
DILATED_PAIRS = ((128, 1), (512, 4), (2048, 16))
ROPE_THETA = 10000.0
HGRN_CHUNK = 64
N_EXPERTS = 256
N_EXPERT_GROUPS = 8
TOPK_GROUPS = 4
TOP_K = 8
EXPERT_DIM = 512
SHARED_DIM = 512
ROUTED_SCALE = 2.5
MOE_BLOCK = 128
ALPHA = (2 * DEPTH) ** 0.25
BETA = (8 * DEPTH) ** -0.25
LN_EPS = 1e-5
RMS_EPS = 1e-6
NEG_INF = -1e30

kernel_name = 'hybrid_dilated_attn_hgrn2_moe_encoder_layer'


def layer_norm(x, g, b):
    xf = x.astype(F32)
    mu = jnp.mean(xf, axis=-1, keepdims=True)
    var = jnp.mean(jnp.square(xf - mu), axis=-1, keepdims=True)
    return ((xf - mu) * lax.rsqrt(var + LN_EPS) * g + b).astype(x.dtype)


def head_rms_norm(t, g):
    tf = t.astype(F32)
    y = tf * lax.rsqrt(jnp.mean(jnp.square(tf), axis=-1, keepdims=True) + RMS_EPS)
    return y * g.astype(F32).reshape(t.shape[-2], t.shape[-1])


def rotary(t, positions):
    half = t.shape[-1] // 2
    inv_freq = ROPE_THETA ** (-jnp.arange(half, dtype=F32) / half)
    ang = positions.astype(F32)[:, None, :, None] * inv_freq
    cos, sin = jnp.cos(ang), jnp.sin(ang)
    t1, t2 = t[..., :half].astype(F32), t[..., half:].astype(F32)
    return jnp.concatenate([t1 * cos - t2 * sin, t2 * cos + t1 * sin], axis=-1)


def banded_local_attention(q, k, v, half):
    *lead, L, dh = q.shape
    blk = half
    nb = -(-L // blk)
    lp = nb * blk
    pad_lead = [(0, 0)] * len(lead)
    qb = jnp.pad(q, pad_lead + [(0, lp - L), (0, 0)]).reshape(*lead, nb, blk, dh)

    def band(t):
        tb = jnp.pad(t, pad_lead + [(blk, lp - L + blk), (0, 0)]).reshape(*lead, nb + 2, blk, dh)
        return jnp.concatenate([tb[..., :-2, :, :], tb[..., 1:-1, :, :], tb[..., 2:, :, :]], axis=-2)

    kb, vb = band(k), band(v)
    s = jnp.einsum('...nqd,...nkd->...nqk', qb, kb).astype(F32) * (dh ** -0.5)
    q_pos = jnp.arange(nb)[:, None, None] * blk + jnp.arange(blk)[None, :, None]
    k_pos = jnp.arange(nb)[:, None, None] * blk - blk + jnp.arange(3 * blk)[None, None, :]
    mask = (jnp.abs(k_pos - q_pos) <= half) & (k_pos >= 0) & (k_pos < L)
    s = jnp.where(mask, s, NEG_INF)
    m = jnp.max(s, axis=-1, keepdims=True)
    p = jnp.exp(s - m)
    l = jnp.sum(p, axis=-1, keepdims=True)
    o = jnp.einsum('...nqk,...nkd->...nqd', (p / l).astype(vb.dtype), vb)
    lse = (m + jnp.log(l))[..., 0]
    return o.reshape(*lead, lp, dh)[..., :L, :], lse.reshape(*lead, lp)[..., :L]


def to_strided(t, dilation):
    B, H, S, dh = t.shape
    return t.reshape(B, H, S // dilation, dilation, dh).transpose(0, 1, 3, 2, 4)


def dilated_window_attention(q, k, v):
    B, H, S, dh = q.shape
    outs, lses = [], []
    for window, dilation in DILATED_PAIRS:
        half = window // (2 * dilation)
        o, lse = banded_local_attention(to_strided(q, dilation), to_strided(k, dilation),
                                        to_strided(v, dilation), half)
        outs.append(o.transpose(0, 1, 3, 2, 4).reshape(B, H, S, dh).astype(F32))
        lses.append(lse.transpose(0, 1, 3, 2).reshape(B, H, S))
    w = jax.nn.softmax(jnp.stack(lses), axis=0)
    return jnp.einsum('rbhs,rbhsd->bhsd', w, jnp.stack(outs))


def hgrn2_bidirectional(q, f_fwd, f_bwd, v, lb_fwd, lb_bwd):
    B, S, H, dh = q.shape
    C = HGRN_CHUNK
    N = S // C

    def gates(z, lb):
        lb = lb.astype(F32).reshape(H, dh)
        zf = z.astype(F32)
        return jnp.log(lb + (1.0 - lb) * jax.nn.sigmoid(zf)), (1.0 - lb) * jax.nn.sigmoid(-zf)

    logf_fw, k_fw = gates(f_fwd, lb_fwd)
    logf_bw, k_bw = gates(jnp.flip(f_bwd, axis=1), lb_bwd)
    qf, vf = q.astype(F32), v.astype(F32)

    def chunks(fw, bw):
        t = jnp.stack([fw, bw])
        return t.reshape(2, B, N, C, H, dh).transpose(2, 0, 1, 4, 3, 5)

    xs = (chunks(qf, jnp.flip(qf, axis=1)), chunks(k_fw, k_bw),
          chunks(vf, jnp.flip(vf, axis=1)), chunks(logf_fw, logf_bw))
    tril = jnp.tril(jnp.ones((C, C), dtype=bool))

    def chunk_step(state, inp):
        qc, kc, vc, lfc = inp
        b = jnp.cumsum(lfc, axis=-2)
        diff = b[..., :, None, :] - b[..., None, :, :]
        decay = jnp.exp(jnp.where(tril[:, :, None], diff, -jnp.inf))
        scores = jnp.einsum('gbhtk,gbhsk,gbhtsk->gbhts', qc, kc, decay)
        o = (jnp.einsum('gbhts,gbhsv->gbhtv', scores, vc)
             + jnp.einsum('gbhtk,gbhkv->gbhtv', qc * jnp.exp(b), state))
        b_last = b[..., -1:, :]
        state = (jnp.exp(b_last[..., 0, :])[..., :, None] * state
                 + jnp.einsum('gbhsk,gbhsv->gbhkv', kc * jnp.exp(b_last - b), vc))
        return state, o

    state0 = jnp.zeros((2, B, H, dh, dh), F32)
    _, o = lax.scan(chunk_step, state0, xs)
    o = o.transpose(1, 2, 0, 4, 3, 5).reshape(2, B, S, H, dh)
    return o[0] + jnp.flip(o[1], axis=1)


def hybrid_mixer(u, positions, w_in, lb_fwd, lb_bwd, attn_norm_g, hgrn_norm_g, w_out):
    B, S, _ = u.shape
    proj = jnp.einsum('bsd,dp->bsp', u, w_in)
    a, h = ATTN_WIDTH, HGRN_WIDTH
    split_at = [a, 2 * a, 3 * a, 3 * a + h, 3 * a + 2 * h, 3 * a + 3 * h, 3 * a + 4 * h]
    qa, ka, va, qh, ffw, fbw, vh, gh = jnp.split(proj, split_at, axis=-1)

    def attn_heads(t):
        return t.reshape(B, S, ATTN_HEADS, ATTN_HEAD_DIM).transpose(0, 2, 1, 3)

    def hgrn_heads(t):
        return t.reshape(B, S, HGRN_HEADS, HGRN_HEAD_DIM)

    attn = dilated_window_attention(rotary(attn_heads(qa), positions),
                                    rotary(attn_heads(ka), positions), attn_heads(va))
    attn = head_rms_norm(attn.transpose(0, 2, 1, 3), attn_norm_g).reshape(B, S, ATTN_WIDTH)
    rec = hgrn2_bidirectional(hgrn_heads(qh), hgrn_heads(ffw), hgrn_heads(fbw), hgrn_heads(vh),
                              lb_fwd, lb_bwd)
    rec = (head_rms_norm(rec, hgrn_norm_g)
           * jax.nn.silu(hgrn_heads(gh).astype(F32))).reshape(B, S, HGRN_WIDTH)
    mixed = jnp.concatenate([attn, rec], axis=-1).astype(u.dtype)
    return jnp.einsum('bsm,md->bsd', mixed, w_out)


def swiglu(t, wg, wu, wd):
    return (jax.nn.silu(t @ wg) * (t @ wu)) @ wd


def route(h, w_router, router_bias):
    T = h.shape[0]
    scores = jax.nn.sigmoid(h.astype(F32) @ w_router.astype(F32))
    sel = scores + router_bias.astype(F32)
    grouped = sel.reshape(T, N_EXPERT_GROUPS, N_EXPERTS // N_EXPERT_GROUPS)
    group_score = jnp.sum(lax.top_k(grouped, 2)[0], axis=-1)
    _, gidx = lax.top_k(group_score, TOPK_GROUPS)
    gmask = jnp.sum(jax.nn.one_hot(gidx, N_EXPERT_GROUPS, dtype=F32), axis=-2) > 0
    emask = jnp.repeat(gmask, N_EXPERTS // N_EXPERT_GROUPS, axis=-1)
    _, idx = lax.top_k(jnp.where(emask, sel, -jnp.inf), TOP_K)
    w = jnp.take_along_axis(scores, idx, axis=-1)
    w = w / jnp.sum(w, axis=-1, keepdims=True) * ROUTED_SCALE
    return idx, w


def routed_experts(h, idx, gate_w, w_gate, w_up, w_down):
    T, D = h.shape
    n_assign = T * TOP_K
    n_blocks = -(-(n_assign + N_EXPERTS * (MOE_BLOCK - 1)) // MOE_BLOCK)
    n_pad = n_blocks * MOE_BLOCK
    expert_id = idx.reshape(-1)
    token_id = jnp.repeat(jnp.arange(T, dtype=jnp.int32), TOP_K)
    weight = gate_w.reshape(-1)
    order = jnp.argsort(expert_id)
    e_sorted, t_sorted, w_sorted = expert_id[order], token_id[order], weight[order]
    counts = jnp.bincount(expert_id, length=N_EXPERTS)
    start = jnp.cumsum(counts) - counts
    padded = (counts + MOE_BLOCK - 1) // MOE_BLOCK * MOE_BLOCK
    padded_end = jnp.cumsum(padded)
    dest = padded_end[e_sorted] - padded[e_sorted] + jnp.arange(n_assign) - start[e_sorted]
    tok_buf = jnp.full((n_pad,), T, jnp.int32).at[dest].set(t_sorted)
    w_buf = jnp.zeros((n_pad,), F32).at[dest].set(w_sorted)
    block_expert = jnp.minimum(
        jnp.searchsorted(padded_end, jnp.arange(n_blocks) * MOE_BLOCK, side='right'), N_EXPERTS - 1)
    h_pad = jnp.concatenate([h, jnp.zeros((1, D), h.dtype)], axis=0)

    def block_step(acc, blk):
        tok, wt, e = blk
        y = swiglu(h_pad[tok], w_gate[e], w_up[e], w_down[e]).astype(F32) * wt[:, None]
        return acc.at[tok].add(y), None

    acc, _ = lax.scan(block_step, jnp.zeros((T + 1, D), F32),
                      (tok_buf.reshape(n_blocks, MOE_BLOCK), w_buf.reshape(n_blocks, MOE_BLOCK),
                       block_expert))
    return acc[:T]


def moe_ffn(h, w_router, router_bias, w_gate, w_up, w_down, ws_gate, ws_up, ws_down):
    idx, gate_w = route(h, w_router, router_bias)
    routed = routed_experts(h, idx, gate_w, w_gate, w_up, w_down)
    shared = swiglu(h, ws_gate, ws_up, ws_down).astype(F32)
    return (shared + routed).astype(h.dtype)


def setup_inputs(seed: int = 0) -> dict:
    key = jax.random.key(seed)
    ks = jax.random.split(key, 22)
    D = D_MODEL

    def normal(k, shape, scale):
        return jax.random.normal(k, shape, F32) * scale

    value_cols = jnp.concatenate([
        jnp.ones((2 * ATTN_WIDTH,), F32), jnp.full((ATTN_WIDTH,), BETA, F32),
        jnp.ones((3 * HGRN_WIDTH,), F32), jnp.full((HGRN_WIDTH,), BETA, F32),
        jnp.ones((HGRN_WIDTH,), F32)])
    return {
        'x': normal(ks[0], (BATCH, SEQ, D), 1.0),
        'c': normal(ks[1], (BATCH, D), 1.0),
        'positions': jnp.tile(jnp.arange(SEQ, dtype=jnp.int32)[None, :], (BATCH, 1)),
        'w_ada': normal(ks[2], (DEPTH, D, 6 * D), 0.5 * D ** -0.5),
        'b_ada': normal(ks[3], (DEPTH, 6 * D), 0.02),
        'w_in': normal(ks[4], (DEPTH, D, IN_PROJ_WIDTH), D ** -0.5) * value_cols,
        'lb_logits': normal(ks[5], (2, DEPTH + 1, HGRN_WIDTH), 0.5),
        'attn_norm_g': 1.0 + normal(ks[6], (DEPTH, ATTN_WIDTH), 0.02),
        'hgrn_norm_g': 1.0 + normal(ks[7], (DEPTH, HGRN_WIDTH), 0.02),
        'w_out': normal(ks[8], (DEPTH, MIX_WIDTH, D), BETA * MIX_WIDTH ** -0.5),
        'ln1_g': 1.0 + normal(ks[9], (DEPTH, D), 0.02),
        'ln1_b': normal(ks[10], (DEPTH, D), 0.02),
        'w_router': normal(ks[11], (DEPTH, D, N_EXPERTS), D ** -0.5),
        'router_bias': normal(ks[12], (DEPTH, N_EXPERTS), 0.01),
        'expert_w_gate': normal(ks[13], (DEPTH, N_EXPERTS, D, EXPERT_DIM), D ** -0.5),
        'expert_w_up': normal(ks[14], (DEPTH, N_EXPERTS, D, EXPERT_DIM), D ** -0.5),
        'expert_w_down': normal(ks[15], (DEPTH, N_EXPERTS, EXPERT_DIM, D), BETA * EXPERT_DIM ** -0.5),
        'shared_w_gate': normal(ks[16], (DEPTH, D, SHARED_DIM), D ** -0.5),
        'shared_w_up': normal(ks[17], (DEPTH, D, SHARED_DIM), D ** -0.5),
        'shared_w_down': normal(ks[18], (DEPTH, SHARED_DIM, D), BETA * SHARED_DIM ** -0.5),
        'ln2_g': 1.0 + normal(ks[19], (DEPTH, D), 0.02),
        'ln2_b': normal(ks[20], (DEPTH, D), 0.02),
    }


def reference(x, c, positions, w_ada, b_ada, w_in, lb_logits, attn_norm_g, hgrn_norm_g, w_out,
              ln1_g, ln1_b, w_router, router_bias, expert_w_gate, expert_w_up, expert_w_down,
              shared_w_gate, shared_w_up, shared_w_down, ln2_g, ln2_b):
    B, S, D = x.shape
    lower_bounds = jnp.cumsum(jax.nn.softmax(lb_logits.astype(F32), axis=1), axis=1)
    for layer in range(DEPTH):
        mod = jax.nn.silu(c) @ w_ada[layer] + b_ada[layer]
        shift1, scale1, gate1, shift2, scale2, gate2 = jnp.split(mod[:, None, :], 6, axis=-1)
        u = x * (1.0 + scale1) + shift1
        mix = hybrid_mixer(u, positions, w_in[layer], lower_bounds[0, layer], lower_bounds[1, layer],
                           attn_norm_g[layer], hgrn_norm_g[layer], w_out[layer])
        x = layer_norm(ALPHA * x + gate1 * mix, ln1_g[layer], ln1_b[layer])
        u = x * (1.0 + scale2) + shift2
        ffn = moe_ffn(u.reshape(B * S, D), w_router[layer], router_bias[layer],
                      expert_w_gate[layer], expert_w_up[layer], expert_w_down[layer],
                      shared_w_gate[layer], shared_w_up[layer], shared_w_down[layer]).reshape(B, S, D)
        x = layer_norm(ALPHA * x + gate2 * ffn, ln2_g[layer], ln2_b[layer])
    return x
```

```python
from contextlib import ExitStack
import numpy as np
import ml_dtypes
import concourse.bass as bass
import concourse.mybir as mybir
from concourse.bass_utils import run_bass_kernel_spmd

F32 = mybir.dt.float32
BF16 = mybir.dt.bfloat16
I32 = mybir.dt.int32
AF = mybir.ActivationFunctionType
ALU = mybir.AluOpType
NPBF = ml_dtypes.bfloat16


class Buf:
    def __init__(self, t):
        self.t = t
        self.w = None
        self.r = {}


class Em:
    ENG = ['pe', 'act', 'dve', 'pool', 'sp']
    NDS = 6

    def __init__(self, nc):
        self.nc = nc
        self.q = {e: [] for e in self.ENG}
        self.cnt = {e: 0 for e in self.ENG}
        self.known = {e: {} for e in self.ENG}
        self.sem = {}
        for e in ['pe', 'act', 'dve', 'pool']:
            self.sem[e] = nc.alloc_semaphore('s_' + e)
        self.dtot = {}
        self.drr = {}
        for e in ['sp', 'pool', 'act']:
            self.drr[e] = 0
            for i in range(self.NDS):
                n = 'd_%s_%d' % (e, i)
                self.sem[n] = nc.alloc_semaphore(n)
                self.dtot[n] = 0
        self.ninst = 0
        self._ccs = nc.alloc_sbuf_tensor('cc_scratch', [1, 8], F32)

    def _waits(self, e, reads, writes, extra=()):
        waits = {}

        def need(pos):
            if pos is None:
                return
            k, v = pos
            if k == e and e == 'pe':
                return
            if waits.get(k, 0) < v:
                waits[k] = v
        for b in reads:
            need(b.w)
        for b in writes:
            need(b.w)
            for k, v in b.r.items():
                need((k, v))
        for p in extra:
            need(p)
        wl = [(k, v) for k, v in waits.items() if self.known[e].get(k, 0) < v]
        for k, v in wl:
            self.known[e][k] = v
        return wl

    def _mark(self, pos, reads, writes):
        for b in writes:
            b.w = pos
            b.r = {}
        for b in reads:
            if b.r.get(pos[0], 0) < pos[1]:
                b.r[pos[0]] = pos[1]

    def emit(self, e, fn, reads=(), writes=(), sig=True):
        wl = self._waits(e, reads, writes)
        if sig:
            self.cnt[e] += 1
            pos = (e, self.cnt[e])
        else:
            pos = (e, self.cnt[e] + 1)
        self.q[e].append((fn, wl, self.sem[e] if sig else None, 1))
        self._mark(pos, reads, writes)
        self.ninst += 1
        return pos

    def dma(self, e, out, in_, reads=(), writes=()):
        i = self.drr[e]
        self.drr[e] = (i + 1) % self.NDS
        n = 'd_%s_%d' % (e, i)
        prev = self.dtot[n]
        wl = self._waits(e, reads, writes, extra=[(n, prev)] if prev else [])
        self.dtot[n] = prev + 16
        pos = (n, prev + 16)
        self.q[e].append((lambda eng, o=out, i_=in_: eng.dma_start(out=o, in_=i_), wl, self.sem[n], 16))
        self._mark(pos, reads, writes)
        self.ninst += 1
        return pos

    def coll(self, kind, ins, outs, groups, reads=(), writes=()):
        if 'cc' not in self.sem:
            self.sem['cc'] = self.nc.alloc_semaphore('cc')
            self.ncc = 0
        wl = self._waits('pool', reads, writes)
        op = ALU.add if kind in ("AllReduce", "ReduceScatter") else ALU.bypass
        self.q['pool'].append((lambda eng: eng.collective_compute(kind, op, replica_groups=groups, ins=ins, outs=outs),
                               wl, self.sem['cc'], 1))
        self.ncc += 1
        self.known['pool']['cc'] = self.ncc
        self.cnt['pool'] += 1
        pos = ('pool', self.cnt['pool'])
        ccs = self._ccs
        self.q['pool'].append((lambda eng: eng.memset(ccs[0:1, 0:1], 0.0), [('cc', self.ncc)], self.sem['pool'], 1))
        self._mark(pos, reads, writes)
        return pos

    def barrier(self):
        allpos = [(e, self.cnt[e]) for e in ['pe', 'act', 'dve', 'pool'] if self.cnt[e]]
        allpos += [(n, v) for n, v in self.dtot.items() if v]
        for e in self.ENG:
            wl = [(k, v) for k, v in allpos if k != e and self.known[e].get(k, 0) < v]
            for k, v in wl:
                self.known[e][k] = v
            self.q[e].append((None, wl, None, 0))

    def finalize(self):
        nc = self.nc
        self.barrier()
        sem = self.sem
        with nc.Block() as block:
            def run(eng, q):
                for fn, wl, sg, inc in q:
                    for k, v in wl:
                        eng.wait_ge(sem[k], v)
                    if fn is not None:
                        ins = fn(eng)
                        if sg is not None:
                            ins.then_inc(sg, inc)

            @block.tensor
            def _(eng):
                run(eng, self.q['pe'])

            @block.scalar
            def _(eng):
                run(eng, self.q['act'])

            @block.vector
            def _(eng):
                run(eng, self.q['dve'])

            @block.gpsimd
            def _(eng):
                run(eng, self.q['pool'])

            @block.sync
            def _(eng):
                run(eng, self.q['sp'])

    def mm(self, out, lhsT, rhs, start, stop, reads, writes, sig=None):
        if sig is None:
            sig = stop
        return self.emit('pe', lambda eng: eng.matmul(out, lhsT, rhs, start=start, stop=stop),
                         reads, writes, sig=sig)

    def act(self, out, in_, func, reads, writes, bias=None, scale=None, accum_out=None):
        kw = {}
        if bias is not None:
            kw['bias'] = bias
        if scale is not None:
            kw['scale'] = scale
        if accum_out is not None:
            kw['accum_out'] = accum_out
        return self.emit('act', lambda eng: eng.activation(out, in_, func, **kw), reads, writes)

    def ts(self, e, out, in0, s1, s2, op0, op1, reads, writes):
        if op1 is None:
            return self.emit(e, lambda eng: eng.tensor_scalar(out, in0, s1, None, op0), reads, writes)
        return self.emit(e, lambda eng: eng.tensor_scalar(out, in0, s1, s2, op0, op1), reads, writes)

    def tt(self, e, out, in0, in1, op, reads, writes):
        return self.emit(e, lambda eng: eng.tensor_tensor(out, in0, in1, op), reads, writes)

    def stt(self, out, in0, scalar, in1, op0, op1, reads, writes):
        return self.emit('dve', lambda eng: eng.scalar_tensor_tensor(out, in0, scalar, in1, op0, op1),
                         reads, writes)

    def copy(self, e, out, in_, reads, writes):
        if e == 'act':
            return self.emit('act', lambda eng: eng.activation(out, in_, AF.Copy), reads, writes)
        return self.emit(e, lambda eng: eng.tensor_copy(out, in_), reads, writes)

    def memset(self, e, ap, val, writes):
        return self.emit(e, lambda eng: eng.memset(ap, val), (), writes)


def sap(T, dims, off=0, p0=0, np_=128):
    R = 1
    for s in T.shape[1:]:
        R *= s
    return bass.AP(T, p0 * R + off, [[R, np_]] + [list(d) for d in dims])


S = 8192
D = 2048
EPS = 1e-6
PI = float(np.pi)


def consts_l1():
    c = {}
    p = np.arange(128)
    c['invf'] = (10000.0 ** (-(p % 32) / 32.0)).astype(np.float32)[:, None]
    c['sgn'] = (np.where((p % 64) < 32, -1.0, 1.0) * 0.999999).astype(np.float32)[:, None]
    perm = np.zeros((128, 128), np.float32)
    for i in range(128):
        j = (i // 64) * 64 + ((i % 64) + 32) % 64
        perm[j, i] = 1.0
    c['perm'] = perm.astype(NPBF)
    c['ident'] = np.eye(128, dtype=np.float32).astype(NPBF)
    dl = np.arange(20)[:, None, None]
    pp = np.arange(128)[None, :, None]
    cc = np.arange(512)[None, None, :]
    dlt = (dl - 8) * 128 + pp - cc
    w = (np.abs(dlt) <= 64).astype(np.float32) + ((dlt % 4 == 0) & (np.abs(dlt) <= 256)) + \
        ((dlt % 16 == 0) & (np.abs(dlt) <= 1024))
    c['amask'] = np.ascontiguousarray(w.transpose(1, 0, 2)).astype(NPBF)
    s_ = np.arange(128)[:, None]
    t_ = np.arange(128)[None, :]
    same = (s_ // 64) == (t_ // 64)
    hm = np.stack([(same & (s_ <= t_)), (same & (s_ >= t_))], axis=1).astype(np.float32)
    c['hmask'] = hm.astype(NPBF)
    rm = np.ones((128, 2048), np.float32)
    rm[:, ::64] = 0.0
    c['rmask'] = rm
    wn = np.ones((65, 64), np.float32)
    wn[64, :] = 64.0 * EPS
    c['wn'] = wn
    c['ones'] = np.ones((128, 128), np.float32).astype(NPBF)
    return c


def emit_l1(nc, em, ps, bps, m1, b_m1, mixT, b_mix, phases=('b', 'c', 'd')):
    din = lambda n, s, d: nc.dram_tensor(n, s, d, kind="ExternalInput")
    xT = din("xT", [D, S], F32).ap()
    win = din("win", [D, 2048], F32).ap()
    posd = din("pos", [1, S], I32)
    lbl = din("lbl", [128, 2, 2, 2], F32).ap()
    ang_d = din("ang", [64, 4], F32).ap()
    hng_d = din("hng", [128, 2], F32).ap()
    cd = {k: din(k, list(v.shape), {np.dtype('float32'): F32}.get(v.dtype, BF16)).ap() for k, v in consts_l1().items()}
    projT = nc.dram_tensor("projT", [8 * 128, S], BF16).ap()
    zT = nc.dram_tensor("zT", [4 * 128, S], F32).ap()
    tokm = nc.dram_tensor("tokm", [S, 512], BF16).ap()
    b_projT, b_zT, b_tokm = Buf(None), Buf(None), Buf(None)
    A = nc.alloc_sbuf_tensor

    def ld(name, shape, dt, src):
        t = A("s_" + name, shape, dt)
        b = Buf(t)
        em.dma('sp', t[:], src, (), [b])
        return t, b
    invf, b_invf = ld("invf", [128, 1], F32, cd['invf'])
    sgn, b_sgn = ld("sgn", [128, 1], F32, cd['sgn'])
    perm, b_perm = ld("perm", [128, 128], BF16, cd['perm'])
    ident, b_ident = ld("ident", [128, 128], BF16, cd['ident'])
    ones, b_ones = ld("ones", [128, 128], BF16, cd['ones'])
    hmask, b_hmask = ld("hmask", [128, 2, 128], BF16, cd['hmask'])
    wn, b_wn = ld("wn", [65, 64], F32, cd['wn'])
    lb_l, b_lbl = ld("lb_l", [128, 2, 2, 2], F32, lbl)
    ang, b_ang = ld("ang", [64, 4], F32, ang_d)
    hng, b_hng = ld("hng", [128, 2], F32, hng_d)
    sc1p = A("sc1p", [128, 16], F32)
    b_sc1p = Buf(sc1p)
    em.ts('dve', sc1p[:], m1[:, 16:32], 1.0, None, ALU.add, None, [b_m1], [b_sc1p])
    lbd = A("lbd", [128, 2, 2], F32); b_lbd = Buf(lbd)
    lb = A("lb", [128, 2, 2], F32); b_lb = Buf(lb)
    oml = A("oml", [128, 2, 2], F32); b_oml = Buf(oml)
    em.tt('dve', lbd[:], lb_l[:, :, :, 0], lb_l[:, :, :, 1], ALU.subtract, [b_lbl], [b_lbd])
    em.act(lb[:], lbd[:], AF.Sigmoid, [b_lbd], [b_lb])
    em.ts('dve', oml[:], lb[:], -1.0, 1.0, ALU.mult, ALU.add, [b_lb], [b_oml])

    if 'b' in phases:
        with ExitStack() as _st:
            W16 = _st.enter_context(nc.sbuf_tensor("W16", [128, 16, 2048], BF16))
            xs0 = _st.enter_context(nc.sbuf_tensor("xs0", [128, 16, 512], F32))
            xs1 = _st.enter_context(nc.sbuf_tensor("xs1", [128, 16, 512], F32))
            u0 = _st.enter_context(nc.sbuf_tensor("u0", [128, 16, 512], BF16))
            u1 = _st.enter_context(nc.sbuf_tensor("u1", [128, 16, 512], BF16))
            posi = _st.enter_context(nc.sbuf_tensor("posi", [128, 512], I32))
            posf = _st.enter_context(nc.sbuf_tensor("posf", [128, 512], F32))
            a1 = _st.enter_context(nc.sbuf_tensor("a1", [128, 512], F32))
            a2 = _st.enter_context(nc.sbuf_tensor("a2", [128, 512], F32))
            ni = _st.enter_context(nc.sbuf_tensor("ni", [128, 512], I32))
            cosT = _st.enter_context(nc.sbuf_tensor("cosT", [128, 512], F32))
            sinT = _st.enter_context(nc.sbuf_tensor("sinT", [128, 512], F32))
            t16 = _st.enter_context(nc.sbuf_tensor("t16", [128, 2, 512], BF16))
            r1 = _st.enter_context(nc.sbuf_tensor("r1", [128, 2, 512], F32))
            o16 = _st.enter_context(nc.sbuf_tensor("o16", [128, 4, 512], BF16))
            o32 = _st.enter_context(nc.sbuf_tensor("o32", [128, 2, 512], F32))
            bW = Buf(W16)
            bxs = Buf(xs0)
            bxs1 = Buf(xs1)
            winv = win.rearrange("(k p) c -> p k c", p=128)
            for cg in range(4):
                for kh in range(2):
                    em.dma('sp', xs0[:, kh * 8:(kh + 1) * 8, :], winv[:, kh * 8:(kh + 1) * 8, cg * 512:(cg + 1) * 512], (), [bxs])
                for kh in range(4):
                    e = ['dve', 'pool', 'act', 'dve'][kh]
                    em.copy(e, W16[:, kh * 4:(kh + 1) * 4, cg * 512:(cg + 1) * 512], xs0[:, kh * 4:(kh + 1) * 4, :], [bxs], [bW])
            bu = [Buf(u0), Buf(u1)]
            us = [u0, u1]
            b_posi, b_posf, b_a1, b_a2, b_ni, b_cos, b_sin = [Buf(t) for t in (posi, posf, a1, a2, ni, cosT, sinT)]
            b_t16 = [Buf(None), Buf(None)]
            b_r1 = [Buf(None), Buf(None)]
            b_o16 = [Buf(None) for _ in range(4)]
            b_o32 = [Buf(None) for _ in range(2)]
            xv = xT.rearrange("(k p) t -> p k t", p=128)
            psi = 0
            oi = 0
            o32i = 0
            ri = 0
            fm_tiles = [0, 1, 2, 3, 8, 9, 10, 11, 12, 13, 14, 15]
            for tt in range(16):
                t0 = tt * 512
                u = us[tt % 2]
                b_u = bu[tt % 2]
                xsc, bxc = (xs0, bxs) if tt % 2 == 0 else (xs1, bxs1)
                if tt == 0:
                    for kq in range(4):
                        em.dma('sp', xs0[:, kq * 4:(kq + 1) * 4, :], xv[:, kq * 4:(kq + 1) * 4, 0:512], (), [bxs])
                if tt + 1 < 16:
                    xsn, bxn = (xs1, bxs1) if tt % 2 == 0 else (xs0, bxs)
                    for kq in range(4):
                        em.dma('sp', xsn[:, kq * 4:(kq + 1) * 4, :], xv[:, kq * 4:(kq + 1) * 4, t0 + 512:t0 + 1024], (), [bxn])
                em.dma('sp', posi[:], bass.AP(posd, t0, [[0, 128], [1, 512]]), (), [b_posi])
                for k in range(16):
                    if k % 2 == 0:
                        em.act(u[:, k, :], xsc[:, k, :], AF.Identity, [bxc, b_sc1p, b_m1], [b_u],
                               bias=m1[:, k:k + 1], scale=sc1p[:, k:k + 1])
                    else:
                        em.ts('dve', u[:, k, :], xsc[:, k, :], sc1p[:, k:k + 1], m1[:, k:k + 1], ALU.mult, ALU.add,
                              [bxc, b_sc1p, b_m1], [b_u])
                em.copy('dve', posf[:], posi[:], [b_posi], [b_posf])
                em.ts('dve', a1[:], posf[:], invf[:, 0:1], None, ALU.mult, None, [b_posf, b_invf], [b_a1])
                for ph, dst, bdst in ((0.0, sinT, b_sin), (PI / 2, cosT, b_cos)):
                    em.ts('dve', a2[:], a1[:], ph, float(1 / (2 * PI)), ALU.add, ALU.mult, [b_a1], [b_a2])
                    em.copy('dve', ni[:], a2[:], [b_a2], [b_ni])
                    em.copy('dve', a2[:], ni[:], [b_ni], [b_a2])
                    em.stt(a2[:], a2[:], float(-2 * PI), a1[:], ALU.mult, ALU.add, [b_a2, b_a1], [b_a2])
                    if ph == 0.0:
                        em.act(dst[:], a2[:], AF.Sin, [b_a2, b_sgn], [bdst], scale=sgn[:, 0:1])
                    else:
                        em.act(dst[:], a2[:], AF.Sin, [b_a2], [bdst], bias=float(PI / 2 * 0.999999), scale=0.999999)
                for ci, ct in enumerate(fm_tiles):
                    pt = ps[psi % 4]; bpt = bps[psi % 4]; psi += 1
                    for k in range(16):
                        em.mm(pt[:], W16[:, k, ct * 128:(ct + 1) * 128], u[:, k, :], k == 0, k == 15, [bW, b_u], [bpt])
                    if ct < 4:
                        j = ri % 2; ri += 1
                        em.copy('act', t16[:, j, :], pt[:], [bpt], [b_t16[j]])
                        p2 = ps[4 + j]; bp2 = bps[4 + j]
                        em.mm(p2[:], perm[:], t16[:, j, :], True, True, [b_perm, b_t16[j]], [bp2])
                        em.tt('dve', r1[:, j, :], t16[:, j, :], cosT[:], ALU.mult, [b_t16[j], b_cos], [b_r1[j]])
                        em.tt('dve', o32[:, j, :], p2[:], sinT[:], ALU.mult, [bp2, b_sin], [b_o32[j]])
                        oj = oi % 4; oi += 1
                        em.tt('pool', o16[:, oj, :], r1[:, j, :], o32[:, j, :], ALU.add, [b_r1[j], b_o32[j]], [b_o16[oj]])
                        em.dma('pool', projT[ct * 128:(ct + 1) * 128, t0:t0 + 512], o16[:, oj, :], [b_o16[oj]], [b_projT])
                    elif 10 <= ct <= 13:
                        j = o32i % 2; o32i += 1
                        em.copy('act' if j else 'dve', o32[:, j, :], pt[:], [bpt], [b_o32[j]])
                        zi = ct - 10
                        em.dma('pool', zT[zi * 128:(zi + 1) * 128, t0:t0 + 512], o32[:, j, :], [b_o32[j]], [b_zT])
                    else:
                        oj = oi % 4; oi += 1
                        em.copy('act' if oj % 2 else 'dve', o16[:, oj, :], pt[:], [bpt], [b_o16[oj]])
                        pi_ = {8: 4, 9: 5, 14: 6, 15: 7}[ct]
                        em.dma('pool', projT[pi_ * 128:(pi_ + 1) * 128, t0:t0 + 512], o16[:, oj, :], [b_o16[oj]], [b_projT])
                for sub in range(4):
                    pt = ps[6 + sub % 2]; bpt = bps[6 + sub % 2]
                    for k in range(16):
                        em.mm(pt[:], u[:, k, sub * 128:(sub + 1) * 128], W16[:, k, 512:1024], k == 0, k == 15, [bW, b_u], [bpt])
                    oj = oi % 4; oi += 1
                    em.copy('act' if sub % 2 else 'dve', o16[:, oj, :], pt[:], [bpt], [b_o16[oj]])
                    em.dma('pool', tokm[t0 + sub * 128:t0 + (sub + 1) * 128, :], o16[:, oj, :], [b_o16[oj]], [b_tokm])
        em.barrier()

    if 'c' in phases:
        with ExitStack() as _st:
            QT = _st.enter_context(nc.sbuf_tensor("QT", [128, S], BF16))
            KT = _st.enter_context(nc.sbuf_tensor("KT", [128, S], BF16))
            Va = _st.enter_context(nc.sbuf_tensor("Va", [128, 64, 2, 65], BF16))
            amask = _st.enter_context(nc.sbuf_tensor("amask_s", [128, 20, 512], BF16))
            PT = _st.enter_context(nc.sbuf_tensor("PT", [128, 3, 512], BF16))
            PM = _st.enter_context(nc.sbuf_tensor("PM", [128, 3, 512], BF16))
            Oq = _st.enter_context(nc.sbuf_tensor("Oq", [65, 2, 512], F32))
            sq = _st.enter_context(nc.sbuf_tensor("sq", [65, 2, 512], F32))
            lnv = _st.enter_context(nc.sbuf_tensor("lnv", [64, 2, 512], F32))
            on = _st.enter_context(nc.sbuf_tensor("on", [64, 2, 512], BF16))
            b_am = Buf(amask)
            em.dma('sp', amask[:, 0:10, :], cd['amask'][:, 0:10, :], (), [b_am])
            em.dma('sp', amask[:, 10:20, :], cd['amask'][:, 10:20, :], (), [b_am])
            bQ, bK, bV = Buf(QT), Buf(KT), Buf(Va)
            bPT = [Buf(None) for _ in range(3)]
            bPM = [Buf(None) for _ in range(3)]
            bOq = [Buf(None) for _ in range(2)]
            bsq = [Buf(None) for _ in range(2)]
            bln = [Buf(None) for _ in range(2)]
            bon = [Buf(None) for _ in range(2)]
            it = 0
            un = 0
            for hp in range(2):
                em.dma('sp', QT[:], projT[hp * 128:(hp + 1) * 128, :], [b_projT], [bQ])
                em.dma('sp', KT[:], projT[(2 + hp) * 128:(3 + hp) * 128, :], [b_projT], [bK])
                em.memset('pool', Va[:], 1.0, [bV])
                tv = tokm.rearrange("(kt p) c -> p kt c", p=128)
                for h in range(2):
                    for kq in range(4):
                        em.dma('sp', Va[:, kq * 16:(kq + 1) * 16, h, 0:64],
                               tv[:, kq * 16:(kq + 1) * 16, hp * 128 + h * 64: hp * 128 + (h + 1) * 64], [b_tokm], [bV])
                for qs in range(16):
                    for h in range(2):
                        kts = list(range(max(0, 4 * qs - 8), min(63, 4 * qs + 11) + 1))
                        j = un % 2; un += 1
                        pO = ps[4 + j]; bpO = bps[4 + j]
                        for ki, kt in enumerate(kts):
                            r = it % 3; it += 1
                            pS = ps[r]; bpS = bps[r]
                            em.mm(pS[:], KT[h * 64:(h + 1) * 64, kt * 128:(kt + 1) * 128],
                                  QT[h * 64:(h + 1) * 64, qs * 512:(qs + 1) * 512], True, True, [bK, bQ], [bpS])
                            em.act(PT[:, r, :], pS[:], AF.Exp, [bpS], [bPT[r]], scale=0.125)
                            em.tt('dve', PM[:, r, :], PT[:, r, :], amask[:, kt - 4 * qs + 8, :], ALU.mult, [bPT[r], b_am], [bPM[r]])
                            em.mm(pO[0:65, :], Va[:, kt, h, :], PM[:, r, :], ki == 0, ki == len(kts) - 1, [bV, bPM[r]], [bpO])
                        em.copy('act', Oq[:, j, :], pO[0:65, :], [bpO], [bOq[j]])
                        em.act(sq[:, j, :], Oq[:, j, :], AF.Square, [bOq[j]], [bsq[j]])
                        pN = ps[6 + j]; bpN = bps[6 + j]
                        em.mm(pN[0:64, :], wn[:], sq[:, j, :], True, True, [b_wn, bsq[j]], [bpN])
                        em.act(lnv[:, j, :], pN[0:64, :], AF.Ln, [bpN], [bln[j]], scale=1.0 / 64)
                        em.act(lnv[:, j, :], lnv[:, j, :], AF.Exp, [bln[j]], [bln[j]], scale=-0.5)
                        hh = hp * 2 + h
                        em.stt(on[:, j, :], Oq[0:64, j, :], ang[:, hh:hh + 1], lnv[:, j, :], ALU.mult, ALU.mult,
                               [bOq[j], b_ang, bln[j]], [bon[j]])
                        em.dma('pool', mixT[hh * 64:(hh + 1) * 64, qs * 512:(qs + 1) * 512], on[:, j, :], [bon[j]], [b_mix])
        em.barrier()

    if 'd' in phases:
        SG = 2048
        with ExitStack() as _st:
            Ofw = _st.enter_context(nc.sbuf_tensor("Ofw", [128, S], F32))
            Vt = _st.enter_context(nc.sbuf_tensor("Vt", [128, 64, 128], BF16))
            rmask = _st.enter_context(nc.sbuf_tensor("rmask_s", [128, SG], F32))
            zs = _st.enter_context(nc.sbuf_tensor("zs", [128, SG], F32))
            qsg = _st.enter_context(nc.sbuf_tensor("qs_", [128, SG], BF16))
            ff = _st.enter_context(nc.sbuf_tensor("ff", [128, SG], F32))
            lf = _st.enter_context(nc.sbuf_tensor("lf", [128, SG], F32))
            bb = _st.enter_context(nc.sbuf_tensor("bb", [128, SG], F32))
            b2 = _st.enter_context(nc.sbuf_tensor("b2", [128, SG], F32))
            rb = _st.enter_context(nc.sbuf_tensor("rb", [128, SG], F32))
            kT = _st.enter_context(nc.sbuf_tensor("kT", [128, SG], F32))
            ktl = _st.enter_context(nc.sbuf_tensor("ktl", [128, SG], BF16))
            qtl = _st.enter_context(nc.sbuf_tensor("qtl", [128, SG], BF16))
            khT = _st.enter_context(nc.sbuf_tensor("khT", [128, SG], BF16))
            dec = _st.enter_context(nc.sbuf_tensor("dec", [128, 32], F32))
            khat = _st.enter_context(nc.sbuf_tensor("khat", [128, 2, 128], BF16))
            Am = _st.enter_context(nc.sbuf_tensor("Am", [128, 2, 128], BF16))
            S32 = _st.enter_context(nc.sbuf_tensor("S32", [128, 2, 128], F32))
            S16 = _st.enter_context(nc.sbuf_tensor("S16", [128, 3, 128], BF16))
            gsg = _st.enter_context(nc.sbuf_tensor("gsg", [128, 512], BF16))
            gsl = _st.enter_context(nc.sbuf_tensor("gsl", [128, 512], F32))
            sq2 = _st.enter_context(nc.sbuf_tensor("sq2", [128, 512], BF16))
            ln2 = _st.enter_context(nc.sbuf_tensor("ln2", [128, 512], F32))
            t1 = _st.enter_context(nc.sbuf_tensor("t1", [128, 512], F32))
            on2 = _st.enter_context(nc.sbuf_tensor("on2", [128, 2, 512], BF16))
            (b_Ofw, b_Vt, b_rm, b_zs, b_qsg, b_ff, b_lf, b_bb, b_b2, b_rb, b_kT, b_ktl, b_qtl, b_khT, b_dec) = \
                [Buf(t) for t in (Ofw, Vt, rmask, zs, qsg, ff, lf, bb, b2, rb, kT, ktl, qtl, khT, dec)]
            b_khat = [Buf(None), Buf(None)]
            b_Am = [Buf(None), Buf(None)]
            b_S32 = [Buf(None), Buf(None)]
            b_S16 = [Buf(None) for _ in range(3)]
            b_gsg, b_gsl, b_sq2, b_ln2, b_t1 = [Buf(t) for t in (gsg, gsl, sq2, ln2, t1)]
            b_on2 = [Buf(None), Buf(None)]
            em.dma('sp', rmask[:], cd['rmask'], (), [b_rm])
            tv = tokm.rearrange("(kt p) c -> p kt c", p=128)
            tile_i = 0
            for h in range(2):
                for kq in range(4):
                    em.dma('sp', Vt[:, kq * 16:(kq + 1) * 16, :], tv[:, kq * 16:(kq + 1) * 16, 256 + h * 128:256 + (h + 1) * 128],
                           [b_tokm], [b_Vt])
                for dr in range(2):
                    s32i = 0
                    s16i = 0
                    em.memset('dve', S32[:, 0, :], 0.0, [b_S32[0]])
                    em.memset('pool', S16[:, 0, :], 0.0, [b_S16[0]])
                    segs = range(4) if dr == 0 else range(3, -1, -1)
                    for sg in segs:
                        c0 = sg * SG
                        em.dma('sp', zs[:], zT[(dr * 2 + h) * 128:(dr * 2 + h + 1) * 128, c0:c0 + SG], [b_zT], [b_zs])
                        em.dma('sp', qsg[:], projT[(4 + h) * 128:(5 + h) * 128, c0:c0 + SG], [b_projT], [b_qsg])
                        em.act(ff[:], zs[:], AF.Sigmoid, [b_zs], [b_ff])
                        em.ts('dve', ff[:], ff[:], oml[:, dr, h:h + 1], lb[:, dr, h:h + 1], ALU.mult, ALU.add, [b_ff, b_oml, b_lb], [b_ff])
                        em.act(lf[:], ff[:], AF.Ln, [b_ff], [b_lf])
                        em.emit('dve', lambda eng: eng.tensor_tensor_scan(bb[:], rmask[:], lf[:], 0.0, ALU.mult, ALU.add),
                                [b_rm, b_lf], [b_bb])
                        totb = sap(bb, [[64, 32], [0, 64]], off=63)
                        v3 = lambda T: sap(T, [[64, 32], [1, 64]])
                        em.tt('dve', v3(rb), totb, v3(bb), ALU.subtract, [b_bb], [b_rb])
                        if dr == 0:
                            bq, brw, b_bq, b_brw = bb, rb, b_bb, b_rb
                        else:
                            em.tt('dve', b2[:], rb[:], lf[:], ALU.add, [b_rb, b_lf], [b_b2])
                            em.tt('dve', rb[:], bb[:], lf[:], ALU.subtract, [b_bb, b_lf, b_b2], [b_rb])
                            bq, brw, b_bq, b_brw = b2, rb, b_b2, b_rb
                        em.act(dec[:], sap(bb, [[64, 32]], off=63), AF.Exp, [b_bb], [b_dec])
                        em.ts('dve', kT[:], ff[:], -1.0, 1.0, ALU.mult, ALU.add, [b_ff], [b_kT])
                        em.act(lf[:], bq[:], AF.Exp, [b_bq], [b_lf])
                        em.tt('dve', qtl[:], qsg[:], lf[:], ALU.mult, [b_qsg, b_lf], [b_qtl])
                        em.act(ff[:], bq[:], AF.Exp, [b_bq, b_kT], [b_ff], scale=-1.0)
                        em.tt('dve', ktl[:], kT[:], ff[:], ALU.mult, [b_kT, b_ff], [b_ktl])
                        em.act(lf[:], brw[:], AF.Exp, [b_brw, b_qtl], [b_lf])
                        em.tt('dve', khT[:], kT[:], lf[:], ALU.mult, [b_kT, b_lf], [b_khT])
                        tiles = range(16) if dr == 0 else range(15, -1, -1)
                        for tl in tiles:
                            gt = sg * 16 + tl
                            cs = slice(tl * 128, (tl + 1) * 128)
                            j = tile_i % 2; tile_i += 1
                            pK = ps[0 + j]; bpK = bps[0 + j]
                            em.mm(pK[:, 0:128], khT[:, cs], ident[:], True, True, [b_khT, b_ident], [bpK])
                            em.copy('act', khat[:, j, :], pK[:, 0:128], [bpK], [b_khat[j]])
                            pA = ps[2 + j]; bpA = bps[2 + j]
                            em.mm(pA[:, 0:128], ktl[:, cs], qtl[:, cs], True, True, [b_ktl, b_qtl], [bpA])
                            em.tt('dve', Am[:, j, :], pA[:, 0:128], hmask[:, dr, :], ALU.mult, [bpA, b_hmask], [b_Am[j]])
                            pO = ps[4 + j]; bpO = bps[4 + j]
                            em.mm(pO[:, 0:128], Vt[:, gt, :], Am[:, j, :], True, False, [b_Vt, b_Am[j]], [bpO], sig=False)
                            chunks = (0, 1) if dr == 0 else (1, 0)
                            for ci, c in enumerate(chunks):
                                cc = slice(tl * 128 + c * 64, tl * 128 + (c + 1) * 64)
                                si = s16i % 3
                                em.mm(pO[:, c * 64:(c + 1) * 64], S16[:, si, :], qtl[:, cc], False, ci == 1,
                                      [b_S16[si], b_qtl], [bpO], sig=(ci == 1))
                                pKV = ps[6 + (s32i % 2)]; bpKV = bps[6 + (s32i % 2)]
                                em.mm(pKV[:, 0:128], khat[c * 64:(c + 1) * 64, j, :], Vt[c * 64:(c + 1) * 64, gt, :], True, True,
                                      [b_khat[j], b_Vt], [bpKV])
                                so, sn = s32i % 2, (s32i + 1) % 2
                                ch = tl * 2 + c
                                em.stt(S32[:, sn, :], S32[:, so, :], dec[:, ch:ch + 1], pKV[:, 0:128], ALU.mult, ALU.add,
                                       [b_S32[so], b_dec, bpKV], [b_S32[sn]])
                                s32i += 1
                                s16i += 1
                                em.copy('act', S16[:, s16i % 3, :], S32[:, sn, :], [b_S32[sn]], [b_S16[s16i % 3]])
                            ocs = slice(gt * 128, (gt + 1) * 128)
                            if dr == 0:
                                em.copy('act', Ofw[:, ocs], pO[:, 0:128], [bpO], [b_Ofw])
                            else:
                                em.tt('dve', Ofw[:, ocs], pO[:, 0:128], Ofw[:, ocs], ALU.add, [bpO, b_Ofw], [b_Ofw])
                for tt in range(16):
                    cs = slice(tt * 512, (tt + 1) * 512)
                    j = tt % 2
                    em.dma('sp', gsg[:], projT[(6 + h) * 128:(7 + h) * 128, cs], [b_projT], [b_gsg])
                    em.act(sq2[:], Ofw[:, cs], AF.Square, [b_Ofw], [b_sq2])
                    pN = ps[j]; bpN = bps[j]
                    em.mm(pN[:], ones[:], sq2[:], True, True, [b_ones, b_sq2], [bpN])
                    em.ts('dve', ln2[:], pN[:], 1.0 / 128, EPS, ALU.mult, ALU.add, [bpN], [b_ln2])
                    em.act(ln2[:], ln2[:], AF.Ln, [b_ln2], [b_ln2])
                    em.act(ln2[:], ln2[:], AF.Exp, [b_ln2], [b_ln2], scale=-0.5)
                    em.act(gsl[:], gsg[:], AF.Silu, [b_gsg], [b_gsl])
                    em.stt(t1[:], Ofw[:, cs], hng[:, h:h + 1], ln2[:], ALU.mult, ALU.mult, [b_Ofw, b_hng, b_ln2], [b_t1])
                    em.tt('dve', on2[:, j, :], t1[:], gsl[:], ALU.mult, [b_t1, b_gsl], [b_on2[j]])
                    em.dma('pool', mixT[256 + h * 128:256 + (h + 1) * 128, cs], on2[:, j, :], [b_on2[j]], [b_mix])
        em.barrier()
    return


def l1_inputs(inp, core):
    b, g = core // 4, core % 4
    w_in = inp['w_in'][0]
    a = 1024

    def cols(base, width, n):
        return list(range(base + g * n * width, base + (g + 1) * n * width))
    order = (cols(0, 64, 4) + cols(a, 64, 4) + cols(2 * a, 64, 4) + cols(3 * a + 3 * 1024, 128, 2) +
             cols(3 * a, 128, 2) + cols(3 * a + 1024, 128, 2) + cols(3 * a + 2048, 128, 2) + cols(3 * a + 4096, 128, 2))
    m = {}
    m['xT'] = np.ascontiguousarray(inp['x'][b].T)
    m['win'] = np.ascontiguousarray(w_in[:, order])
    m['pos'] = np.ascontiguousarray(inp['positions'][b][None, :])
    lbl = inp['lb_logits']
    hs = lbl[:, :, g * 256:(g + 1) * 256].reshape(2, 2, 2, 128)
    m['lbl'] = np.ascontiguousarray(hs.transpose(3, 0, 2, 1))
    m['ang'] = np.ascontiguousarray(inp['attn_norm_g'][0][g * 256:(g + 1) * 256].reshape(4, 64).T)
    m['hng'] = np.ascontiguousarray(inp['hgrn_norm_g'][0][g * 256:(g + 1) * 256].reshape(2, 128).T)
    m.update(consts_l1())
    return m


ALPHA = 2.0 ** 0.25
LN_EPS = 1e-5
NE = 32
NT = 16384
G4 = [[0, 1, 2, 3], [4, 5, 6, 7]]
G2 = [[0, 4], [1, 5], [2, 6], [3, 7]]


def build_fused(ne=NE, ntg=NT // 512, stop=None, skip_l1=False, p2tiles=16):
    nc = bass.Bass("TRN2", target_bir_lowering=False)
    em = Em(nc)
    din = lambda n, s, d: nc.dram_tensor(n, s, d, kind="ExternalInput")
    dint = lambda n, s, d: nc.dram_tensor(n, s, d).ap()
    ps = [nc.alloc_psum_tensor("ps%d" % i, [128, 512], F32) for i in range(8)]
    bps = [Buf(t) for t in ps]
    A = nc.alloc_sbuf_tensor

    def _finish(src_ap, shape, dtype, buf):
        o = nc.dram_tensor("dbg", shape, dtype, kind="ExternalOutput").ap()
        em.dma('sp', o, src_ap, [buf], [Buf(None)])
        em.finalize()
        return nc

    cT = din("cT", [128, 16, 2], F32).ap()
    wada = din("wada", [2048, 1536], F32).ap()
    bada = din("bada", [128, 12], F32).ap()
    bsel_d = din("bsel", [128, 2], F32).ap()
    m_in = dint("m_in", [128, 24], F32)
    m_g4 = dint("m_g4", [512, 24], F32)
    m_all = dint("m_all", [1024, 24], F32)
    b_min, b_mg4, b_mall = Buf(None), Buf(None), Buf(None)
    modb = A("modb", [128, 96], F32); b_modb = Buf(modb)
    bsel = A("bsel_s", [128, 2], F32); b_bsel = Buf(bsel)
    em.dma('sp', bsel[:], bsel_d, (), [b_bsel])
    with ExitStack() as st:
        T = lambda n, s, d: st.enter_context(nc.sbuf_tensor(n, s, d))
        cs = T("cs", [128, 16, 2], F32); bcs = Buf(cs)
        sc = T("sc", [128, 16, 2], F32); bsc = Buf(sc)
        W = T("Wad", [128, 16, 1536], F32); bW = Buf(W)
        bs = T("bs", [128, 12], F32); bbs = Buf(bs)
        o = T("mo", [128, 12, 2], F32); bo = Buf(o)
        mall = T("mall", [128, 8, 12, 2], F32); bmall = Buf(mall)
        em.dma('sp', cs[:], cT, (), [bcs])
        em.dma('sp', bs[:], bada, (), [bbs])
        wv = wada.rearrange("(k p) c -> p k c", p=128)
        for kq in range(4):
            em.dma('sp', W[:, kq * 4:(kq + 1) * 4, :], wv[:, kq * 4:(kq + 1) * 4, :], (), [bW])
        em.act(sc[:], cs[:], AF.Silu, [bcs], [bsc])
        for jt in range(12):
            for k in range(16):
                em.mm(ps[0][:, jt * 2:jt * 2 + 2], W[:, k, jt * 128:(jt + 1) * 128], sc[:, k, :], k == 0, k == 15, [bW, bsc], [bps[0]])
        em.tt('dve', o[:], sap(ps[0], [[2, 12], [1, 2]]), sap(bs, [[1, 12], [0, 2]]), ALU.add, [bps[0], bbs], [bo])
        em.dma('sp', m_in, sap(o, [[1, 24]]), [bo], [b_min])
        em.coll("AllGather", [m_in], [m_g4], G4, [b_min], [b_mg4])
        em.coll("AllGather", [m_g4], [m_all], G2, [b_mg4], [b_mall])
        em.dma('sp', sap(mall, [[24, 8], [1, 24]]), m_all.rearrange("(r p) c -> p r c", p=128), [b_mall], [bmall])
        mv = lambda bi: sap(mall, [[2, 96]], off=bi)
        em.ts('dve', modb[:], mv(0), bsel[:, 0:1], None, ALU.mult, None, [bmall, b_bsel], [b_modb])
        em.stt(modb[:], mv(1), bsel[:, 1:2], modb[:], ALU.mult, ALU.add, [bmall, b_bsel, b_modb], [b_modb])
    em.barrier()

    if stop == 'P0':
        return _finish(modb[:], [128, 96], F32, b_modb)
    mixT_i = dint("mixT_i", [512, S], BF16)
    b_mix = Buf(None)
    if not skip_l1:
        emit_l1(nc, em, ps, bps, modb, b_modb, mixT_i, b_mix)

    wout_g = din("wout_g", [512, 2048], F32).ap()
    ypart = dint("ypart", [S, 2048], F32)
    yq = dint("yq", [2048, 2048], F32)
    b_yp, b_yq = Buf(None), Buf(None)
    with ExitStack() as st:
        T = lambda n, s, d: st.enter_context(nc.sbuf_tensor(n, s, d))
        stg = T("wo_stg", [128, 4, 2048], F32); bstg = Buf(stg)
        Wo = T("Wo", [128, 4, 2048], BF16); bWo = Buf(Wo)
        mx = [T("mxe%d" % i, [128, 4, 512], BF16) for i in range(2)]
        bmx = [Buf(t) for t in mx]
        yo = [T("yo%d" % i, [128, 2048], F32) for i in range(2)]
        byo = [Buf(t) for t in yo]
        em.dma('sp', stg[:], wout_g.rearrange("(k p) c -> p k c", p=128), (), [bstg])
        for k in range(4):
            em.copy(['dve', 'pool', 'act', 'dve'][k], Wo[:, k, :], stg[:, k, :], [bstg], [bWo])
        mvv = mixT_i.rearrange("(k p) t -> p k t", p=128)
        pi = 0
        for tt in range(0 if skip_l1 else 16):
            j = tt % 2
            em.dma('sp', mx[j][:], mvv[:, :, tt * 512:(tt + 1) * 512], [b_mix], [bmx[j]])
            for sub in range(4):
                jo = (tt * 4 + sub) % 2
                for dc in range(4):
                    pY = ps[pi % 4]; bpY = bps[pi % 4]; pi += 1
                    for k in range(4):
                        em.mm(pY[:], mx[j][:, k, sub * 128:(sub + 1) * 128], Wo[:, k, dc * 512:(dc + 1) * 512], k == 0, k == 3,
                              [bmx[j], bWo], [bpY])
                    em.copy('act' if dc % 2 else 'dve', yo[jo][:, dc * 512:(dc + 1) * 512], pY[:], [bpY], [byo[jo]])
                r0 = tt * 512 + sub * 128
                em.dma('pool', ypart[r0:r0 + 128, :], yo[jo][:], [byo[jo]], [b_yp])
    em.coll("ReduceScatter", [ypart], [yq], G4, [b_yp], [b_yq])
    em.barrier()

    if stop == 'P1e':
        return _finish(yq, [2048, 2048], F32, b_yq)
    xq = din("xq", [2048, 2048], F32).ap()
    l1g = din("l1g", [128, 2048], F32).ap()
    l1b = din("l1b", [128, 2048], F32).ap()
    wr = din("wr", [2048, 256], F32).ap()
    rbias = din("rbias", [128, 256], F32).ap()
    x1_i = dint("x1_i", [2048, 2048], F32)
    Uc = [dint("Uc%d" % kk, [128, 2048], BF16) for kk in range(16)]
    Gc = [dint("Gc%d" % j, [512, 256], F32) for j in range(4)]
    b_x1i, b_u2i, b_Gi = Buf(None), Buf(None), Buf(None)
    identF = A("identF", [128, 128], F32); b_idF = Buf(identF)
    g2row = A("g2row", [128, 2048], F32); b_g2row = Buf(g2row)
    with ExitStack() as st:
        T = lambda n, s, d: st.enter_context(nc.sbuf_tensor(n, s, d))
        id16 = T("id16", [128, 128], BF16); bid16 = Buf(id16)
        bct = T("bct", [128, 2, 128], F32); bbct = [Buf(None), Buf(None)]
        g1row = T("g1row", [128, 2048], F32); bg1 = Buf(g1row)
        sh2row = T("sh2row", [128, 2048], F32); bsh2 = Buf(sh2row)
        sc2row = T("sc2row", [128, 2048], F32); bsc2 = Buf(sc2row)
        lgr = T("lgr", [128, 2048], F32); blgr = Buf(lgr)
        lbr = T("lbr", [128, 2048], F32); blbr = Buf(lbr)
        Wr = T("Wr", [128, 16, 256], F32); bWr = Buf(Wr)
        rb_ = T("rb_", [128, 256], F32); brb = Buf(rb_)
        yt = T("yt", [128, 2048], F32); byt = Buf(yt)
        xt = T("xt", [128, 2048], F32); bxt = Buf(xt)
        rr = T("rr", [128, 2048], F32); brr = Buf(rr)
        u2t = T("u2t", [128, 2048], F32); bu2t = Buf(u2t)
        u2f = T("u2f", [128, 16, 128], F32); bu2f = Buf(u2f)
        u2h = T("u2h", [128, 16, 128], BF16); bu2h = Buf(u2h)
        s1 = T("s1", [128, 1], F32); bs1 = Buf(s1)
        s2 = T("s2", [128, 1], F32); bs2 = Buf(s2)
        mean = T("mean", [128, 1], F32); bmean = Buf(mean)
        var = T("var", [128, 1], F32); bvar = Buf(var)
        scr = T("scr", [128, 256], F32); bscr = Buf(scr)
        sel = T("sel", [128, 256], F32); bsel_ = Buf(sel)
        m8 = T("m8", [128, 8, 8], F32); bm8 = Buf(m8)
        gs = T("gs", [128, 8], F32); bgs = Buf(gs)
        g8 = T("g8", [128, 8], F32); bg8 = Buf(g8)
        gm = T("gm", [128, 8], F32); bgm = Buf(gm)
        gt_ = T("gt_", [128, 8], F32); bgt = Buf(gt_)
        t8 = T("t8", [128, 8], F32); bt8 = Buf(t8)
        wsel = T("wsel", [128, 256], F32); bwsel = Buf(wsel)
        ssum = T("ssum", [128, 1], F32); bssum = Buf(ssum)
        Go = T("Go", [128, 2, 256], F32); bGo = [Buf(None), Buf(None)]
        cdid = nc.dram_tensor("identb", [128, 128], BF16, kind="ExternalInput").ap()
        em.dma('sp', id16[:], cdid, (), [bid16])
        em.copy('dve', identF[:], id16[:], [bid16], [b_idF])
        em.dma('sp', lgr[:], l1g, (), [blgr]); em.dma('sp', lbr[:], l1b, (), [blbr])
        em.dma('sp', rb_[:], rbias, (), [brb])
        em.dma('sp', Wr[:], wr.rearrange("(k p) c -> p k c", p=128), (), [bWr])
        pbi = 0
        for (dst, bd, ch0, addone) in ((g1row, bg1, 32, False), (sh2row, bsh2, 48, False), (sc2row, bsc2, 64, True),
                                       (g2row, b_g2row, 80, False)):
            for c4 in range(4):
                pB = ps[pbi % 2]; bpB = bps[pbi % 2]; pbi += 1
                for cc in range(4):
                    c = c4 * 4 + cc
                    jb_ = c % 2
                    em.copy('dve', bct[:, jb_, :], sap(modb, [[0, 128]], off=ch0 + c), [b_modb], [bbct[jb_]])
                    em.mm(pB[:, cc * 128:(cc + 1) * 128], bct[:, jb_, :], identF[:], True, True,
                          [bbct[jb_], b_idF], [bpB])
                if addone:
                    em.ts('dve', dst[:, c4 * 512:(c4 + 1) * 512], pB[:], 1.0, None, ALU.add, None, [bpB], [bd])
                else:
                    em.copy('dve', dst[:, c4 * 512:(c4 + 1) * 512], pB[:], [bpB], [bd])
        gi = 0
        if stop == 'P2a':
            return _finish(g1row[:], [128, 2048], F32, bg1)
        for tl in range(p2tiles):
            r0 = tl * 128
            em.dma('sp', yt[:], yq[r0:r0 + 128, :], [b_yq], [byt])
            em.dma('sp', xt[:], xq[r0:r0 + 128, :], (), [bxt])
            em.tt('dve', yt[:], yt[:], g1row[:], ALU.mult, [byt, bg1], [byt])
            em.stt(rr[:], xt[:], float(ALPHA), yt[:], ALU.mult, ALU.add, [bxt, byt], [brr])
            em.memset('pool', s1[:], 0.0, [bs1]); em.memset('pool', s2[:], 0.0, [bs2])
            em.act(u2t[:], rr[:], AF.Identity, [brr], [bu2t, bs1], accum_out=s1[:])
            em.act(u2t[:], rr[:], AF.Square, [brr], [bu2t, bs2], accum_out=s2[:])
            em.ts('dve', mean[:], s1[:], 1.0 / 2048, None, ALU.mult, None, [bs1], [bmean])
            em.tt('dve', var[:], mean[:], mean[:], ALU.mult, [bmean], [bvar])
            em.stt(var[:], s2[:], 1.0 / 2048, var[:], ALU.mult, ALU.subtract, [bs2, bvar], [bvar])
            em.act(var[:], var[:], AF.Ln, [bvar], [bvar], bias=float(LN_EPS))
            em.act(var[:], var[:], AF.Exp, [bvar], [bvar], scale=-0.5)
            em.ts('dve', rr[:], rr[:], mean[:, 0:1], var[:, 0:1], ALU.subtract, ALU.mult, [brr, bmean, bvar], [brr])
            em.tt('dve', rr[:], rr[:], lgr[:], ALU.mult, [brr, blgr], [brr])
            em.tt('pool', rr[:], rr[:], lbr[:], ALU.add, [brr, blbr], [brr])
            em.dma('pool', x1_i[r0:r0 + 128, :], rr[:], [brr], [b_x1i])
            import os
            cut = int(os.environ.get('P2CUT', '9'))
            if cut <= 1:
                continue
            em.tt('dve', u2t[:], rr[:], sc2row[:], ALU.mult, [brr, bsc2], [bu2t])
            em.tt('dve', u2t[:], u2t[:], sh2row[:], ALU.add, [bu2t, bsh2], [bu2t])
            if cut <= 2:
                continue
            for c4 in range(4):
                pT = ps[2 + c4 % 2]; bpT = bps[2 + c4 % 2]
                for cc in range(4):
                    c = c4 * 4 + cc
                    em.mm(pT[:, cc * 128:(cc + 1) * 128], u2t[:, c * 128:(c + 1) * 128], identF[:], True, True, [bu2t, b_idF], [bpT])
                em.copy('act', sap(u2f, [[1, 512]], off=c4 * 512), pT[:], [bpT], [bu2f])
                em.copy('dve', sap(u2h, [[1, 512]], off=c4 * 512), sap(u2f, [[1, 512]], off=c4 * 512), [bu2f], [bu2h])
            if cut <= 3:
                continue
            for kk in range(16):
                em.dma('pool' if kk % 2 else 'act', Uc[kk][:, r0:r0 + 128], u2h[:, kk, :], [bu2h], [b_u2i])
            if cut <= 4:
                continue
            pR = ps[4 + tl % 2]; bpR = bps[4 + tl % 2]
            for k in range(16):
                em.mm(pR[:, 0:256], u2f[:, k, :], Wr[:, k, :], k == 0, k == 15, [bu2f, bWr], [bpR])
            em.act(scr[:], pR[:, 0:256], AF.Sigmoid, [bpR], [bscr])
            em.tt('dve', sel[:], scr[:], rb_[:], ALU.add, [bscr, brb], [bsel_])
            for g in range(8):
                em.emit('dve', lambda eng, g=g: eng.max(m8[:, g, :], sel[:, g * 32:(g + 1) * 32]), [bsel_], [bm8])
            em.tt('dve', gs[:], m8[:, :, 0], m8[:, :, 1], ALU.add, [bm8], [bgs])
            em.emit('dve', lambda eng: eng.max(g8[:], gs[:]), [bgs], [bg8])
            em.ts('dve', gm[:], gs[:], g8[:, 3:4], None, ALU.is_ge, None, [bgs, bg8], [bgm])
            em.ts('dve', gt_[:], gm[:], 4.0, -4.0, ALU.mult, ALU.add, [bgm], [bgt])
            v3 = lambda X: sap(X, [[32, 8], [1, 32]])
            bc = lambda X: sap(X, [[1, 8], [0, 32]])
            em.tt('dve', v3(sel), v3(sel), bc(gm), ALU.mult, [bsel_, bgm], [bsel_])
            em.tt('dve', v3(sel), v3(sel), bc(gt_), ALU.add, [bsel_, bgt], [bsel_])
            em.emit('dve', lambda eng: eng.max(t8[:], sel[:]), [bsel_], [bt8])
            em.ts('dve', wsel[:], sel[:], t8[:, 7:8], None, ALU.is_ge, None, [bsel_, bt8], [bwsel])
            em.tt('dve', wsel[:], wsel[:], scr[:], ALU.mult, [bwsel, bscr], [bwsel])
            em.emit('dve', lambda eng: eng.reduce_sum(ssum[:], wsel[:], mybir.AxisListType.X), [bwsel], [bssum])
            em.emit('dve', lambda eng: eng.reciprocal(ssum[:], ssum[:]), [bssum], [bssum])
            j = gi % 2; gi += 1
            em.ts('dve', Go[:, j, :], wsel[:], ssum[:, 0:1], 2.5, ALU.mult, ALU.mult, [bwsel, bssum], [bGo[j]])
            em.dma('pool', Gc[tl // 4][(tl % 4) * 128:(tl % 4 + 1) * 128, :], Go[:, j, :], [bGo[j]], [b_Gi])
    if stop == 'P2':
        return _finish(x1_i, [2048, 2048], F32, b_x1i)
    if stop == 'P2G':
        return _finish(Gc[0], [512, 256], F32, b_Gi)
    UA = [dint("UA%d" % kk, [512, 2048], BF16) for kk in range(16)]
    UB = [dint("UB%d" % kk, [1024, 2048], BF16) for kk in range(16)]
    GA = [dint("GA%d" % j, [2048, 256], F32) for j in range(4)]
    GB = [dint("GB%d" % j, [4096, 256], F32) for j in range(4)]
    b_u2g4, b_u2all, b_Gg4, b_Gall = Buf(None), Buf(None), Buf(None), Buf(None)
    for kk in range(16):
        em.coll("AllGather", [Uc[kk]], [UA[kk]], G4, [b_u2i], [b_u2g4])
    for kk in range(16):
        em.coll("AllGather", [UA[kk]], [UB[kk]], G2, [b_u2g4], [b_u2all])
    for j in range(4):
        em.coll("AllGather", [Gc[j]], [GA[j]], G4, [b_Gi], [b_Gg4])
    for j in range(4):
        em.coll("AllGather", [GA[j]], [GB[j]], G2, [b_Gg4], [b_Gall])
    em.barrier()

    if stop == 'AG':
        return _finish(GB[1], [4096, 256], F32, b_Gall)
    gsel_d = din("gsel", [128, 8], F32).ap()
    wg = din("wg", [ne, 2048, 512], F32).ap()
    wu = din("wu", [ne, 2048, 512], F32).ap()
    wd = din("wd", [ne, 512, 2048], F32).ap()
    wg16 = dint("wg16", [ne, 128, 8192], BF16)
    wu16 = dint("wu16", [ne, 128, 8192], BF16)
    wd16 = dint("wd16", [ne, 128, 8192], BF16)
    Yp_i = dint("Yp_i", [NT, 2048], F32)
    y2 = dint("y2", [NT // 2, 2048], F32)
    yr = dint("yr", [2048, 2048], F32)
    b_w16, bY, b_y2, b_yr = Buf(None), Buf(None), Buf(None), Buf(None)
    with ExitStack() as st:
        T = lambda n, s, d: st.enter_context(nc.sbuf_tensor(n, s, d))
        stg = [T("stg%d" % i, [128, 16, 512], F32) for i in range(2)]
        bstg = [Buf(t) for t in stg]
        o16 = [T("c16_%d" % i, [128, 16, 512], BF16) for i in range(2)]
        bo16 = [Buf(t) for t in o16]
        i = 0
        for e in range(ne):
            for src, dst, pat in ((wg, wg16, 0), (wu, wu16, 0), (wd, wd16, 1)):
                j = i % 2; i += 1
                sv = src[e].rearrange("(k p) c -> p k c", p=128)
                dv = dst[e]
                if pat == 0:
                    em.dma('sp', stg[j][:, 0:8, :], sv[:, 0:8, :], (), [bstg[j]])
                    em.dma('sp', stg[j][:, 8:16, :], sv[:, 8:16, :], (), [bstg[j]])
                else:
                    em.dma('sp', sap(stg[j], [[2048, 2], [1, 2048]]), sv[:, 0:2, :], (), [bstg[j]])
                    em.dma('sp', sap(stg[j], [[2048, 2], [1, 2048]], off=4096), sv[:, 2:4, :], (), [bstg[j]])
                for q in range(4):
                    eng = ['dve', 'pool', 'act', 'pool'][q]
                    em.copy(eng, o16[j][:, q * 4:(q + 1) * 4, :], stg[j][:, q * 4:(q + 1) * 4, :], [bstg[j]], [bo16[j]])
                em.dma('pool', dv, sap(o16[j], [[1, 8192]]), [bo16[j]], [b_w16])
    em.barrier()
    with ExitStack() as st:
        T = lambda n, s, d: st.enter_context(nc.sbuf_tensor(n, s, d))
        gsel = T("gsel_s", [128, 8], F32); bgsel = Buf(gsel)
        em.dma('sp', gsel[:], gsel_d, (), [bgsel])
        u2 = [T("u2_%d" % i, [128, 16, 512], BF16) for i in range(2)]
        bu2 = [Buf(t) for t in u2]
        Gf = [T("Gf%d" % i, [128, 4, 256], F32) for i in range(2)]
        bGf = [Buf(t) for t in Gf]
        Gt = [T("Gt%d" % i, [128, 4, ne], F32) for i in range(2)]
        bGt = [Buf(t) for t in Gt]
        acc = T("acc0", [128, 4, 2048], F32); bacc = Buf(acc)
        Wg = [T("Wg%d" % i, [128, 16, 512], BF16) for i in range(2)]
        Wu = [T("Wu%d" % i, [128, 16, 512], BF16) for i in range(2)]
        Wd = [T("Wd%d" % i, [128, 4, 2048], BF16) for i in range(2)]
        bWg = [Buf(t) for t in Wg]; bWu = [Buf(t) for t in Wu]; bWd = [Buf(t) for t in Wd]
        sil = T("sil", [128, 2, 512], BF16); bsil = [Buf(None), Buf(None)]
        hT = [T("hT%d" % i, [128, 4, 512], BF16) for i in range(2)]
        bhT = [Buf(t) for t in hT]
        wi = 0
        pgi = 0
        pyi = 0
        for tg in range(ntg):
            jt = tg % 2
            t0 = tg * 512
            rk, off = tg // 4, (tg % 4) * 512
            for kk in range(16):
                em.dma('sp', u2[jt][:, kk, :], UB[kk][rk * 128:(rk + 1) * 128, off:off + 512], [b_u2all], [bu2[jt]])
            em.dma('sp', Gf[jt][:], GB[tg % 4][rk * 512:(rk + 1) * 512, :].rearrange("(s p) e -> p s e", p=128), [b_Gall], [bGf[jt]])
            for jb in range(8):
                src = Gf[jt][:, :, jb * ne:(jb + 1) * ne]
                if jb == 0:
                    em.ts('dve', Gt[jt][:], src, gsel[:, 0:1], None, ALU.mult, None, [bGf[jt], bgsel], [bGt[jt]])
                else:
                    em.stt(Gt[jt][:], src, gsel[:, jb:jb + 1], Gt[jt][:], ALU.mult, ALU.add, [bGf[jt], bgsel, bGt[jt]], [bGt[jt]])
            for e in range(ne):
                jw = wi % 2; wi += 1
                em.dma('sp', sap(Wg[jw], [[1, 8192]]), wg16[e], [b_w16], [bWg[jw]])
                em.dma('sp', sap(Wu[jw], [[1, 8192]]), wu16[e], [b_w16], [bWu[jw]])
                em.dma('sp', sap(Wd[jw], [[1, 8192]]), wd16[e], [b_w16], [bWd[jw]])
                for ft in range(4):
                    pG = ps[pgi % 2]; bpG = bps[pgi % 2]
                    pU = ps[2 + pgi % 2]; bpU = bps[2 + pgi % 2]
                    sj = pgi % 2
                    pgi += 1
                    for k in range(16):
                        em.mm(pG[:], Wg[jw][:, k, ft * 128:(ft + 1) * 128], u2[jt][:, k, :], k == 0, k == 15, [bWg[jw], bu2[jt]], [bpG])
                    for k in range(16):
                        em.mm(pU[:], Wu[jw][:, k, ft * 128:(ft + 1) * 128], u2[jt][:, k, :], k == 0, k == 15, [bWu[jw], bu2[jt]], [bpU])
                    em.act(sil[:, sj, :], pG[:], AF.Silu, [bpG], [bsil[sj]])
                    em.tt('dve', hT[jw][:, ft, :], sil[:, sj, :], pU[:], ALU.mult, [bsil[sj], bpU], [bhT[jw]])
                for sub in range(4):
                    for dc in range(4):
                        pY = ps[4 + pyi % 4]; bpY = bps[4 + pyi % 4]; pyi += 1
                        for ft in range(4):
                            em.mm(pY[:], hT[jw][:, ft, sub * 128:(sub + 1) * 128], Wd[jw][:, ft, dc * 512:(dc + 1) * 512],
                                  ft == 0, ft == 3, [bhT[jw], bWd[jw]], [bpY])
                        a = acc[:, sub, dc * 512:(dc + 1) * 512]
                        if e == 0:
                            em.ts('dve', a, pY[:], Gt[jt][:, sub, e:e + 1], None, ALU.mult, None, [bpY, bGt[jt]], [bacc])
                        else:
                            em.stt(a, pY[:], Gt[jt][:, sub, e:e + 1], a, ALU.mult, ALU.add, [bpY, bGt[jt], bacc], [bacc])
            for sub in range(4):
                em.dma('pool', Yp_i[t0 + sub * 128:t0 + (sub + 1) * 128, :], acc[:, sub, :], [bacc], [bY])
    em.coll("ReduceScatter", [Yp_i], [y2], G2, [bY], [b_y2])
    em.coll("ReduceScatter", [y2], [yr], G4, [b_y2], [b_yr])
    em.barrier()

    if stop == 'P3':
        return _finish(yr, [2048, 2048], F32, b_yr)
    sg = din("sg", [2048, 512], F32).ap()
    su = din("su", [2048, 512], F32).ap()
    sd = din("sd", [512, 2048], F32).ap()
    lg = din("l2g", [128, 2048], F32).ap()
    lb_ = din("l2b", [128, 2048], F32).ap()
    out = nc.dram_tensor("out", [2048, 2048], F32, kind="ExternalOutput").ap()
    bout = Buf(None)
    with ExitStack() as st:
        T = lambda n, s, d: st.enter_context(nc.sbuf_tensor(n, s, d))
        stg = T("stg4", [128, 8, 512], F32); bstg = Buf(stg)
        Sg = T("Sg", [128, 16, 512], BF16); bSg = Buf(Sg)
        Su = T("Su", [128, 16, 512], BF16); bSu = Buf(Su)
        Sd = T("Sd", [128, 4, 2048], BF16); bSd = Buf(Sd)
        lgs = T("lgs", [128, 2048], F32); blg = Buf(lgs)
        lbs = T("lbs", [128, 2048], F32); blb = Buf(lbs)
        u2 = T("u2p4", [128, 16, 128], BF16); bu2 = Buf(u2)
        yp = T("yp", [128, 2048], F32); byp = Buf(yp)
        x1 = T("x1", [128, 2048], F32); bx1 = Buf(x1)
        ffn = T("ffn", [128, 2048], F32); bffn = Buf(ffn)
        sil = T("sil4", [128, 128], BF16); bsil = Buf(sil)
        hT = T("hT4", [128, 4, 128], BF16); bhT = Buf(hT)
        s1 = T("s1b", [128, 1], F32); bs1 = Buf(s1)
        s2 = T("s2b", [128, 1], F32); bs2 = Buf(s2)
        mean = T("meanb", [128, 1], F32); bmean = Buf(mean)
        var = T("varb", [128, 1], F32); bvar = Buf(var)
        ot = T("ot", [128, 2048], F32); bot = Buf(ot)
        em.dma('sp', lgs[:], lg, (), [blg]); em.dma('sp', lbs[:], lb_, (), [blb])
        for src, dstT, bd, pat in ((sg, Sg, bSg, 0), (su, Su, bSu, 0), (sd, Sd, bSd, 1)):
            sv = src.rearrange("(k p) c -> p k c", p=128)
            for hf in range(2):
                if pat == 0:
                    em.dma('sp', stg[:], sv[:, hf * 8:(hf + 1) * 8, :], (), [bstg])
                    em.copy('dve', dstT[:, hf * 8:(hf + 1) * 8, :], stg[:], [bstg], [bd])
                else:
                    sview = sap(stg, [[2048, 2], [1, 2048]])
                    em.dma('sp', sview, sv[:, hf * 2:(hf + 1) * 2, :], (), [bstg])
                    em.copy('dve', dstT[:, hf * 2:(hf + 1) * 2, :], sview, [bstg], [bd])
        for tl in range(16):
            r0 = tl * 128
            for kk in range(16):
                em.dma('sp' if kk % 2 else 'act', u2[:, kk, :], Uc[kk][:, r0:r0 + 128], [b_u2i], [bu2])
            em.dma('sp', yp[:], yr[r0:r0 + 128, :], [b_yr], [byp])
            em.dma('sp', x1[:], x1_i[r0:r0 + 128, :], [b_x1i], [bx1])
            for ft in range(4):
                pG = ps[ft % 2]; bpG = bps[ft % 2]
                pU = ps[2 + ft % 2]; bpU = bps[2 + ft % 2]
                for k in range(16):
                    em.mm(pG[:, 0:128], Sg[:, k, ft * 128:(ft + 1) * 128], u2[:, k, :], k == 0, k == 15, [bSg, bu2], [bpG])
                for k in range(16):
                    em.mm(pU[:, 0:128], Su[:, k, ft * 128:(ft + 1) * 128], u2[:, k, :], k == 0, k == 15, [bSu, bu2], [bpU])
                em.act(sil[:], pG[:, 0:128], AF.Silu, [bpG], [bsil])
                em.tt('dve', hT[:, ft, :], sil[:], pU[:, 0:128], ALU.mult, [bsil, bpU], [bhT])
            for dc in range(4):
                pY = ps[4 + dc]; bpY = bps[4 + dc]
                for ft in range(4):
                    em.mm(pY[:], hT[:, ft, :], Sd[:, ft, dc * 512:(dc + 1) * 512], ft == 0, ft == 3, [bhT, bSd], [bpY])
                em.tt('dve', ffn[:, dc * 512:(dc + 1) * 512], pY[:], yp[:, dc * 512:(dc + 1) * 512], ALU.add, [bpY, byp], [bffn])
            em.tt('dve', ffn[:], ffn[:], g2row[:], ALU.mult, [bffn, b_g2row], [bffn])
            em.stt(ffn[:], x1[:], float(ALPHA), ffn[:], ALU.mult, ALU.add, [bx1, bffn], [bffn])
            em.memset('pool', s1[:], 0.0, [bs1]); em.memset('pool', s2[:], 0.0, [bs2])
            em.act(ot[:], ffn[:], AF.Identity, [bffn], [bot, bs1], accum_out=s1[:])
            em.act(ot[:], ffn[:], AF.Square, [bffn], [bot, bs2], accum_out=s2[:])
            em.ts('dve', mean[:], s1[:], 1.0 / 2048, None, ALU.mult, None, [bs1], [bmean])
            em.tt('dve', var[:], mean[:], mean[:], ALU.mult, [bmean], [bvar])
            em.stt(var[:], s2[:], 1.0 / 2048, var[:], ALU.mult, ALU.subtract, [bs2, bvar], [bvar])
            em.act(var[:], var[:], AF.Ln, [bvar], [bvar], bias=float(LN_EPS))
            em.act(var[:], var[:], AF.Exp, [bvar], [bvar], scale=-0.5)
            em.ts('dve', ot[:], ffn[:], mean[:, 0:1], var[:, 0:1], ALU.subtract, ALU.mult, [bffn, bmean, bvar], [bot])
            em.tt('dve', ot[:], ot[:], lgs[:], ALU.mult, [bot, blg], [bot])
            em.tt('pool', ot[:], ot[:], lbs[:], ALU.add, [bot, blb], [bot])
            em.dma('pool', out[r0:r0 + 128, :], ot[:], [bot], [bout])
    em.finalize()
    return nc


def fused_inputs(I, k):
    b, q = k // 4, k % 4
    ch = lambda v: np.ascontiguousarray(v.reshape(-1, 128).T)
    rows = lambda v: np.ascontiguousarray(np.tile(np.asarray(v)[None, :], (128, 1)))
    m = l1_inputs(I, k)
    c = I['c']
    m['cT'] = np.ascontiguousarray(c.T.reshape(16, 128, 2).transpose(1, 0, 2))
    m['wada'] = np.ascontiguousarray(I['w_ada'][0][:, k * 1536:(k + 1) * 1536])
    m['bada'] = ch(I['b_ada'][0][k * 1536:(k + 1) * 1536])
    bs = np.zeros((128, 2), np.float32); bs[:, b] = 1.0
    m['bsel'] = bs
    perm = np.r_[q * 256:(q + 1) * 256, 1024 + q * 256:1024 + (q + 1) * 256]
    m['wout_g'] = np.ascontiguousarray(I['w_out'][0][perm, :])
    m['xq'] = np.ascontiguousarray(I['x'][b, q * 2048:(q + 1) * 2048, :])
    m['l1g'] = rows(I['ln1_g'][0]); m['l1b'] = rows(I['ln1_b'][0])
    m['wr'] = np.ascontiguousarray(I['w_router'][0])
    m['rbias'] = rows(I['router_bias'][0])
    m['identb'] = np.eye(128, dtype=np.float32).astype(NPBF)
    gs = np.zeros((128, 8), np.float32); gs[:, k] = 1.0
    m['gsel'] = gs
    m['wg'] = I['expert_w_gate'][0][k * 32:(k + 1) * 32]
    m['wu'] = I['expert_w_up'][0][k * 32:(k + 1) * 32]
    m['wd'] = I['expert_w_down'][0][k * 32:(k + 1) * 32]
    m['sg'] = I['shared_w_gate'][0]; m['su'] = I['shared_w_up'][0]; m['sd'] = I['shared_w_down'][0]
    m['l2g'] = rows(I['ln2_g'][0]); m['l2b'] = rows(I['ln2_b'][0])
    return m


def kernel(**inputs):
    I = {k: np.asarray(v) for k, v in inputs.items()}
    nc = build_fused()
    res = run_bass_kernel_spmd(nc, [fused_inputs(I, k) for k in range(8)], core_ids=list(range(8)))
    out = np.zeros((2, 8192, 2048), np.float32)
    for k in range(8):
        out[k // 4, (k % 4) * 2048:(k % 4 + 1) * 2048, :] = res.results[k]["out"]
    return out
```
